# Optimizing a Trainium2 kernel written in Bass

```python
import math
import jax, jax.numpy as jnp
from jax import lax
import numpy as np

D_MODEL = 1024
BATCH = 2
SEQ = 8192
DEPTH = 2

N_MIXERS = 2
EPS = 1e-6
SSM_EXPAND = 2
SSM_D_INNER = SSM_EXPAND * D_MODEL
SSM_HEAD_DIM = 64
SSM_HEADS = SSM_D_INNER // SSM_HEAD_DIM
SSM_GROUPS = 4
SSM_STATE = 128
SSM_CONV = 4
SSM_CHUNK = 128
SSM_GN = SSM_GROUPS * SSM_STATE
SSM_CONV_DIM = SSM_D_INNER + 2 * SSM_GN
SSM_IN_DIM = SSM_D_INNER + SSM_CONV_DIM + SSM_HEADS
ATTN_HEAD_DIM = 64
ATTN_Q_HEADS = D_MODEL // ATTN_HEAD_DIM
ATTN_KV_HEADS = 4
ATTN_Q_PER_KV = ATTN_Q_HEADS // ATTN_KV_HEADS
WINDOW = 128
ATTN_QKV_DIM = (ATTN_Q_HEADS + 2 * ATTN_KV_HEADS) * ATTN_HEAD_DIM
REL_BUCKETS = 32
REL_MAX_DIST = 128
N_EXPERTS = 32
TOP_K = 4
EXPERT_FF = D_MODEL
SWIGLU_ALPHA = 1.702
SWIGLU_LIMIT = 7.0
MOE_BLOCK = 128
N_SSM_LAYERS = (DEPTH + 1) // 2
N_ATTN_LAYERS = DEPTH // 2

kernel_name = "hybrid_ssd_swa_moe_adaln"


def rms_norm(x, g):
    xf = x.astype(jnp.float32)
    xf = xf * lax.rsqrt(jnp.mean(xf * xf, axis=-1, keepdims=True) + EPS)
    return (xf * g.astype(jnp.float32)).astype(x.dtype)


def modulate(x, g, shift, scale):
    return rms_norm(x, g) * (1 + scale[:, None, :]) + shift[:, None, :]


def ssd_chunked(xs, dt, A, Bm, Cm):
    b, s, h, p = xs.shape
    g, n = Bm.shape[2], Bm.shape[3]
    r = h // g
    nc = s // SSM_CHUNK
    X = (xs.astype(jnp.float32) * dt[..., None]).reshape(b, nc, SSM_CHUNK, g, r, p)
    dA = (dt * A).reshape(b, nc, SSM_CHUNK, g, r).transpose(0, 3, 4, 1, 2)
    Bc = Bm.astype(jnp.float32).reshape(b, nc, SSM_CHUNK, g, n)
    Cc = Cm.astype(jnp.float32).reshape(b, nc, SSM_CHUNK, g, n)
    A_cs = jnp.cumsum(dA, axis=-1)
    causal = jnp.tril(jnp.ones((SSM_CHUNK, SSM_CHUNK), dtype=bool))
    seg = A_cs[..., :, None] - A_cs[..., None, :]
    L = jnp.exp(jnp.where(causal, seg, -jnp.inf))
    CB = jnp.einsum("bclgn,bcsgn->bcgls", Cc, Bc)
    y_diag = jnp.einsum("bcgls,bgrcls,bcsgrp->bclgrp", CB, L, X)
    decay_states = jnp.exp(A_cs[..., -1:] - A_cs)
    states = jnp.einsum("bclgn,bgrcl,bclgrp->bcgrpn", Bc, decay_states, X)
    chunk_decay = jnp.exp(A_cs[..., -1])

    def step(h_prev, inp):
        st, dec = inp
        return h_prev * dec[..., None, None] + st, h_prev

    init = jnp.zeros((b, g, r, p, n), jnp.float32)
    _, h_in = lax.scan(step, init, (jnp.moveaxis(states, 1, 0), jnp.moveaxis(chunk_decay, 3, 0)))
    h_in = jnp.moveaxis(h_in, 0, 1)
    y_off = jnp.einsum("bclgn,bcgrpn,bgrcl->bclgrp", Cc, h_in, jnp.exp(A_cs))
    return (y_diag + y_off).reshape(b, s, h, p)


def mamba2_mixer(h, in_w, conv_w, conv_b, dt_bias, A_log, D_skip, norm_g, out_w):
    b, s, _ = h.shape
    zxbcdt = h @ in_w
    z = zxbcdt[..., :SSM_D_INNER]
    xbc = zxbcdt[..., SSM_D_INNER:SSM_D_INNER + SSM_CONV_DIM]
    dt_raw = zxbcdt[..., SSM_D_INNER + SSM_CONV_DIM:]
    xbc = lax.conv_general_dilated(xbc, conv_w[:, None, :].astype(xbc.dtype), (1,), [(SSM_CONV - 1, 0)],
                                   dimension_numbers=("NWC", "WIO", "NWC"),
                                   feature_group_count=SSM_CONV_DIM) + conv_b
    xbc = jax.nn.silu(xbc)
    xs = xbc[..., :SSM_D_INNER].reshape(b, s, SSM_HEADS, SSM_HEAD_DIM)
    Bm = xbc[..., SSM_D_INNER:SSM_D_INNER + SSM_GN].reshape(b, s, SSM_GROUPS, SSM_STATE)
    Cm = xbc[..., SSM_D_INNER + SSM_GN:].reshape(b, s, SSM_GROUPS, SSM_STATE)
    dt = jax.nn.softplus(dt_raw.astype(jnp.float32) + dt_bias.astype(jnp.float32))
    A = -jnp.exp(A_log.astype(jnp.float32))
    y = ssd_chunked(xs, dt, A, Bm, Cm) + D_skip.astype(jnp.float32)[:, None] * xs.astype(jnp.float32)
    y = y.reshape(b, s, SSM_D_INNER) * jax.nn.silu(z.astype(jnp.float32))
    y = y.reshape(b, s, SSM_GROUPS, SSM_D_INNER // SSM_GROUPS)
    y = y * lax.rsqrt(jnp.mean(y * y, axis=-1, keepdims=True) + EPS)
    y = (y.reshape(b, s, SSM_D_INNER) * norm_g.astype(jnp.float32)).astype(h.dtype)
    return y @ out_w


def t5_causal_bucket(dist):
    max_exact = REL_BUCKETS // 2
    d = jnp.maximum(dist, 1).astype(jnp.float32)
    large = max_exact + (jnp.log(d / max_exact) / math.log(REL_MAX_DIST / max_exact)
                         * (REL_BUCKETS - max_exact)).astype(jnp.int32)
    large = jnp.minimum(large, REL_BUCKETS - 1)
    return jnp.where(dist < max_exact, dist, large)


def head_rms(x, g):
    xf = x.astype(jnp.float32)
    return xf * lax.rsqrt(jnp.mean(xf * xf, axis=-1, keepdims=True) + EPS) * g.astype(jnp.float32)


def swa_mixer(h, qkv_w, q_norm_g, k_norm_g, sinks, rel_bias, out_w):
    b, s, _ = h.shape
    nb = s // WINDOW
    qkv = h @ qkv_w
    q_end = ATTN_Q_HEADS * ATTN_HEAD_DIM
    k_end = q_end + ATTN_KV_HEADS * ATTN_HEAD_DIM
    q = head_rms(qkv[..., :q_end].reshape(b, s, ATTN_KV_HEADS, ATTN_Q_PER_KV, ATTN_HEAD_DIM), q_norm_g)
    k = head_rms(qkv[..., q_end:k_end].reshape(b, s, ATTN_KV_HEADS, ATTN_HEAD_DIM), k_norm_g)
    v = qkv[..., k_end:].reshape(b, s, ATTN_KV_HEADS, ATTN_HEAD_DIM).astype(jnp.float32)
    qb = q.reshape(b, nb, WINDOW, ATTN_KV_HEADS, ATTN_Q_PER_KV, ATTN_HEAD_DIM)
    kb = k.reshape(b, nb, WINDOW, ATTN_KV_HEADS, ATTN_HEAD_DIM)
    vb = v.reshape(b, nb, WINDOW, ATTN_KV_HEADS, ATTN_HEAD_DIM)
    pad = ((0, 0), (1, 0), (0, 0), (0, 0), (0, 0))
    kk = jnp.concatenate([jnp.pad(kb, pad)[:, :-1], kb], axis=2)
    vv = jnp.concatenate([jnp.pad(vb, pad)[:, :-1], vb], axis=2)
    scores = jnp.einsum("bnqgrd,bnkgd->bngrqk", qb, kk) * (ATTN_HEAD_DIM ** -0.5)
    qi = jnp.arange(WINDOW)[:, None]
    kj = jnp.arange(2 * WINDOW)[None, :]
    dist = qi + WINDOW - kj
    in_band = (dist >= 0) & (dist < WINDOW)
    key_pos = jnp.arange(nb)[:, None, None] * WINDOW + kj[None] - WINDOW
    valid = in_band[None] & (key_pos >= 0)
    bias = rel_bias.astype(jnp.float32)[t5_causal_bucket(jnp.maximum(dist, 0))]
    bias = bias.transpose(2, 0, 1).reshape(ATTN_KV_HEADS, ATTN_Q_PER_KV, WINDOW, 2 * WINDOW)
    scores = jnp.where(valid[None, :, None, None], scores + bias[None, None], -jnp.inf)
    sink = sinks.astype(jnp.float32).reshape(ATTN_KV_HEADS, ATTN_Q_PER_KV)[None, None, :, :, None, None]
    m = jnp.maximum(jnp.max(scores, axis=-1, keepdims=True), sink)
    p = jnp.exp(scores - m)
    probs = p / (jnp.sum(p, axis=-1, keepdims=True) + jnp.exp(sink - m))
    out = jnp.einsum("bngrqk,bnkgd->bnqgrd", probs, vv)
    out = out.reshape(b, s, ATTN_Q_HEADS * ATTN_HEAD_DIM).astype(h.dtype)
    return out @ out_w


def clamped_swiglu(hb):
    x_glu = jnp.minimum(hb[..., ::2], SWIGLU_LIMIT)
    x_lin = jnp.clip(hb[..., 1::2], -SWIGLU_LIMIT, SWIGLU_LIMIT)
    return x_glu * jax.nn.sigmoid(SWIGLU_ALPHA * x_glu) * (x_lin + 1)


def moe(h, r_w, r_b, w1, b1, w2, b2):
    b, s, d = h.shape
    T = b * s
    xt = h.reshape(T, d)
    logits = (xt @ r_w + r_b).astype(jnp.float32)
    top_val, top_idx = lax.top_k(logits, TOP_K)
    gates = jax.nn.softmax(top_val, axis=-1)
    n_assign = T * TOP_K
    e_flat = top_idx.reshape(-1)
    tok_flat = jnp.arange(n_assign, dtype=jnp.int32) // TOP_K
    order = jnp.argsort(e_flat)
    sorted_e = e_flat[order]
    sorted_tok = tok_flat[order]
    sorted_g = gates.reshape(-1)[order]
    counts = jnp.bincount(e_flat, length=N_EXPERTS)
    padded = ((counts + MOE_BLOCK - 1) // MOE_BLOCK) * MOE_BLOCK
    pend = jnp.cumsum(padded)
    pstart = pend - padded
    ustart = jnp.cumsum(counts) - counts
    pos = pstart[sorted_e] + (jnp.arange(n_assign, dtype=jnp.int32) - ustart[sorted_e])
    cap = n_assign + N_EXPERTS * MOE_BLOCK
    n_blk = cap // MOE_BLOCK
    tok_pad = jnp.full((cap,), T, jnp.int32).at[pos].set(sorted_tok)
    gate_pad = jnp.zeros((cap,), jnp.float32).at[pos].set(sorted_g)
    blk_start = jnp.arange(n_blk, dtype=jnp.int32) * MOE_BLOCK
    blk_e = jnp.minimum(jnp.sum(blk_start[:, None] >= pend[None, :], axis=1), N_EXPERTS - 1)
    x_ext = jnp.concatenate([xt, jnp.zeros((1, d), xt.dtype)], axis=0)
    xg = x_ext[tok_pad].reshape(n_blk, MOE_BLOCK, d)

    def expert_block(args):
        xb, e = args
        hb = clamped_swiglu(xb @ w1[e] + b1[e])
        return hb @ w2[e] + b2[e]

    yg = lax.map(expert_block, (xg, blk_e)).reshape(cap, d)
    yg = yg * gate_pad[:, None].astype(yg.dtype)
    out = jnp.zeros((T + 1, d), yg.dtype).at[tok_pad].add(yg)
    return out[:T].reshape(b, s, d)


def setup_inputs(seed: int = 0) -> dict:
    key = jax.random.key(seed)
    ks = jax.random.split(key, 32)
    f32 = jnp.float32

    def nrm(k, shape, scale):
        return jax.random.normal(k, shape, f32) * scale

    dt0 = jnp.exp(jax.random.uniform(ks[9], (N_SSM_LAYERS, SSM_HEADS), f32,
                                     minval=math.log(1e-3), maxval=math.log(1e-1)))
    return {
        "x": nrm(ks[0], (BATCH, SEQ, D_MODEL), 1.0),
        "c": nrm(ks[1], (BATCH, D_MODEL), 1.0),
        "ada_w": nrm(ks[2], (DEPTH, D_MODEL, 6 * D_MODEL), 0.01),
        "ada_b": nrm(ks[3], (DEPTH, 6 * D_MODEL), 0.01),
        "norm1_g": 1.0 + nrm(ks[4], (DEPTH, D_MODEL), 0.02),
        "norm2_g": 1.0 + nrm(ks[5], (DEPTH, D_MODEL), 0.02),
        "m_in_w": nrm(ks[6], (N_SSM_LAYERS, D_MODEL, SSM_IN_DIM), D_MODEL ** -0.5),
        "m_conv_w": nrm(ks[7], (N_SSM_LAYERS, SSM_CONV, SSM_CONV_DIM), SSM_CONV ** -0.5),
        "m_conv_b": nrm(ks[8], (N_SSM_LAYERS, SSM_CONV_DIM), 0.02),
        "m_dt_bias": dt0 + jnp.log(-jnp.expm1(-dt0)),
        "m_A_log": jnp.log(jax.random.uniform(ks[10], (N_SSM_LAYERS, SSM_HEADS), f32, minval=1.0, maxval=16.0)),
        "m_D": 1.0 + nrm(ks[11], (N_SSM_LAYERS, SSM_HEADS), 0.1),
        "m_norm_g": 1.0 + nrm(ks[12], (N_SSM_LAYERS, SSM_D_INNER), 0.02),
        "m_out_w": nrm(ks[13], (N_SSM_LAYERS, SSM_D_INNER, D_MODEL), SSM_D_INNER ** -0.5),
        "a_qkv_w": nrm(ks[14], (N_ATTN_LAYERS, D_MODEL, ATTN_QKV_DIM), D_MODEL ** -0.5),
        "a_q_norm_g": 1.0 + nrm(ks[15], (N_ATTN_LAYERS, ATTN_HEAD_DIM), 0.02),
        "a_k_norm_g": 1.0 + nrm(ks[16], (N_ATTN_LAYERS, ATTN_HEAD_DIM), 0.02),
        "a_sinks": nrm(ks[17], (N_ATTN_LAYERS, ATTN_Q_HEADS), 0.5),
        "a_out_w": nrm(ks[18], (N_ATTN_LAYERS, ATTN_Q_HEADS * ATTN_HEAD_DIM, D_MODEL),
                        (ATTN_Q_HEADS * ATTN_HEAD_DIM) ** -0.5),
        "rel_bias": nrm(ks[19], (REL_BUCKETS, ATTN_Q_HEADS), 0.5),
        "r_w": nrm(ks[20], (DEPTH, D_MODEL, N_EXPERTS), D_MODEL ** -0.5),
        "r_b": nrm(ks[21], (DEPTH, N_EXPERTS), 0.01),
        "e_w1": nrm(ks[22], (DEPTH, N_EXPERTS, D_MODEL, 2 * EXPERT_FF), D_MODEL ** -0.5),
        "e_b1": nrm(ks[23], (DEPTH, N_EXPERTS, 2 * EXPERT_FF), 0.01),
        "e_w2": nrm(ks[24], (DEPTH, N_EXPERTS, EXPERT_FF, D_MODEL), EXPERT_FF ** -0.5),
        "e_b2": nrm(ks[25], (DEPTH, N_EXPERTS, D_MODEL), 0.01),
    }


def reference(x, c, ada_w, ada_b, norm1_g, norm2_g, m_in_w, m_conv_w, m_conv_b, m_dt_bias, m_A_log, m_D,
              m_norm_g, m_out_w, a_qkv_w, a_q_norm_g, a_k_norm_g, a_sinks, a_out_w, rel_bias,
              r_w, r_b, e_w1, e_b1, e_w2, e_b2):
    c_act = jax.nn.silu(c)
    for i in range(DEPTH):
        mod = c_act @ ada_w[i] + ada_b[i]
        sh1, sc1, g1, sh2, sc2, g2 = jnp.split(mod, 6, axis=-1)
        h = modulate(x, norm1_g[i], sh1, sc1)
        j = i // N_MIXERS
        if i % N_MIXERS == 0:
            y = mamba2_mixer(h, m_in_w[j], m_conv_w[j], m_conv_b[j], m_dt_bias[j], m_A_log[j], m_D[j],
                             m_norm_g[j], m_out_w[j])
        else:
            y = swa_mixer(h, a_qkv_w[j], a_q_norm_g[j], a_k_norm_g[j], a_sinks[j], rel_bias, a_out_w[j])
        x = x + g1[:, None, :] * y
        h = modulate(x, norm2_g[i], sh2, sc2)
        x = x + g2[:, None, :] * moe(h, r_w[i], r_b[i], e_w1[i], e_b1[i], e_w2[i], e_b2[i])
    return x
```

```python
import numpy as np
from contextlib import ExitStack
import concourse.bass as bass
import concourse.mybir as mybir
from concourse.bass_utils import run_bass_kernel_spmd

F32 = mybir.dt.float32
BF16 = mybir.dt.bfloat16
I32 = mybir.dt.int32
U32 = mybir.dt.uint32
AF = mybir.ActivationFunctionType
ALU = mybir.AluOpType

D = 1024
NE = 32
CAP = 512
EPS = 1e-6


DBG_MAXOPS = 0


class StopEmit(Exception):
    pass


class Tok:
    __slots__ = ("sem", "val", "key")

    def __init__(self, sem, val, key):
        self.sem, self.val, self.key = sem, val, key


class Res:
    __slots__ = ("w", "r", "dsem", "dcnt", "name", "excl")

    def __init__(self, name="", excl=False):
        self.excl = excl
        self.w = None
        self.r = {}
        self.dsem = None
        self.dcnt = 0
        self.name = name


class Ctx:
    def __init__(self, nc):
        self.nc = nc
        self.eng = {"pe": nc.tensor, "act": nc.scalar, "dve": nc.vector, "pool": nc.gpsimd, "sp": nc.sync}
        self.esem = {k: nc.alloc_semaphore("es_" + k) for k in self.eng}
        self.ecnt = {k: 0 for k in self.eng}
        self.waited = {k: {} for k in self.eng}
        self.nsem = 0
        self.ninst = 0
        self.bregs = {}
        self.free_dsems = []
        self.stage_res = []

    def bounds_reg(self, v):
        if v not in self.bregs:
            self.bregs[v] = self.nc.gpsimd.to_reg(v)
        return self.bregs[v]

    def new_sem(self):
        self.nsem += 1
        return self.nc.alloc_semaphore("ds_%d" % self.nsem)

    def _wait(self, en, tok):
        if tok is None:
            return
        w = self.waited[en]
        if w.get(tok.key, 0) >= tok.val:
            return
        w[tok.key] = tok.val
        self.eng[en].wait_ge(tok.sem, tok.val)

    def _pre(self, en, reads, writes, deps, ownkey=None):
        for r in reads:
            self._wait(en, r.w)
            if r.excl:
                for t in r.r.values():
                    if t.key != en:
                        self._wait(en, t)
        for wr in writes:
            if wr.w is not None and wr.w.key != ownkey:
                self._wait(en, wr.w)
            for t in wr.r.values():
                self._wait(en, t)
        for d in deps:
            self._wait(en, d)

    def _post(self, tok, reads, writes):
        for wr in writes:
            wr.w = tok
            wr.r = {}
        for r in reads:
            if r.w is tok:
                continue
            old = r.r.get(tok.key)
            if old is None or old.val < tok.val:
                r.r[tok.key] = tok

    def begin_stage(self):
        self.stage_res = []

    def end_stage(self):
        for r in self.stage_res:
            if r.dsem is not None:
                self.free_dsems.append((r.dsem, r.dcnt, r.name))
                r.dsem = None
        self.stage_res = []

    def barrier(self, extra=()):
        toks = [Tok(self.esem[k], self.ecnt[k], k) for k in self.eng if self.ecnt[k] > 0]
        for en in self.eng:
            for t in toks:
                if t.key != en:
                    self._wait(en, t)
            for t in extra:
                self._wait(en, t)

    def op(self, en, fn, reads=(), writes=(), deps=()):
        if DBG_MAXOPS and self.ninst >= DBG_MAXOPS:
            raise StopEmit()
        if DBG_MAXOPS and self.ninst >= DBG_MAXOPS - 40:
            import inspect
            fr = inspect.stack()[1]
            print("OP", self.ninst, en, fr.lineno, (fr.code_context or [""])[0].strip()[:110])
        self._pre(en, reads, writes, deps)
        inst = fn(self.eng[en])
        self.ecnt[en] += 1
        inst.then_inc(self.esem[en], 1)
        tok = Tok(self.esem[en], self.ecnt[en], en)
        self._post(tok, reads, writes)
        self.ninst += 1
        return tok

    def dma(self, en, fn, reads=(), writes=(), deps=(), n=1, sres=None):
        sres = sres if sres is not None else writes[0]
        if sres.dsem is None:
            if self.free_dsems:
                sres.dsem, sres.dcnt, sres.name = self.free_dsems.pop()
            else:
                sres.dsem = self.new_sem()
                sres.dcnt = 0
                sres.name = "dsem%d" % self.nsem
            self.stage_res.append(sres)
        key = sres.name
        self._pre(en, reads, writes, deps, ownkey=key)
        insts = fn(self.eng[en])
        if not isinstance(insts, (list, tuple)):
            insts = [insts]
        for i in insts:
            i.then_inc(sres.dsem, 16)
            sres.dcnt += 16
        tok = Tok(sres.dsem, sres.dcnt, key)
        self._post(tok, reads, writes)
        self.ninst += len(insts)
        return tok


class Ring:
    def __init__(self, nc, name, n, shape, dtype, es=None):
        if es is None:
            self.t = [nc.alloc_sbuf_tensor("%s%d" % (name, i), shape, dtype) for i in range(n)]
        else:
            self.t = [es.enter_context(nc.sbuf_tensor("%s%d" % (name, i), shape, dtype)) for i in range(n)]
        self.r = [Res("%s%d" % (name, i)) for i in range(n)]
        self.i = 0
        self.n = n

    def next(self):
        j = self.i % self.n
        self.i += 1
        return self.t[j], self.r[j]


def bcast_rows(ap1d, nrows):
    return ap1d.partition_broadcast(nrows)


class Consts:
    def __init__(self, cx, kc_ap):
        nc = cx.nc
        self.res = Res("consts")
        self.kc = nc.alloc_sbuf_tensor("c_kc", [128, 512], F32)
        self.ident_f = self.kc[:, 0:128]
        self.trii_f = self.kc[:, 128:256]
        self.ones_f = self.kc[:, 384:512]
        self.kb = nc.alloc_sbuf_tensor("c_kb", [128, 512], BF16)
        self.ident_b = self.kb[:, 0:128]
        self.trii_b = self.kb[:, 128:256]
        self.tris_b = self.kb[:, 256:384]
        self.ones_b = self.kb[:, 384:512]
        r = self.res
        cx.dma("sp", lambda e: e.dma_start(out=self.kc[:], in_=kc_ap), writes=[r])
        cx.op("dve", lambda e: e.tensor_copy(out=self.kb[:], in_=self.kc[:]), reads=[r], writes=[r])


def host_bucket_onehot():
    bt = t5_bucket_table()
    oh = np.zeros((32, 128), np.float32)
    oh[bt, np.arange(128)] = 1.0
    return oh


def host_consts():
    i = np.arange(128)
    ident = (i[:, None] == i[None, :]).astype(np.float32)
    trii = (i[:, None] <= i[None, :]).astype(np.float32)
    tris = (i[:, None] < i[None, :]).astype(np.float32)
    ones = np.ones((128, 128), np.float32)
    return np.ascontiguousarray(np.concatenate([ident, trii, tris, ones], axis=1))


def emit_adaln(cx, K, cT_ap, ada_w_l, ada_b_l, pieces, psA, psB):
    nc = cx.nc
    uid = "ad%d_" % cx.ninst
    with ExitStack() as es:
        cT = es.enter_context(nc.sbuf_tensor(uid + "cT", [128, 8], F32))
        cA = es.enter_context(nc.sbuf_tensor(uid + "cA", [128, 8], F32))
        rc = Res("ad_c")
        cx.dma("sp", lambda e: e.dma_start(out=cT[:], in_=cT_ap), writes=[rc])
        cx.op("act", lambda e: e.activation(out=cA[:], in_=cT[:], func=AF.Silu), reads=[rc], writes=[rc])
        wring = Ring(nc, uid + "w", 2, [128, 8, 512], F32, es)
        rowring = Ring(nc, uid + "row", 2, [1, 512], F32, es)
        bring = Ring(nc, uid + "b", 2, [1, 512], F32, es)
        for (pi, dst, dres, mode, aux, auxres) in pieces:
            for half in range(2):
                col0 = pi * 1024 + half * 512
                wt, wr = wring.next()
                src = ada_w_l[:, col0:col0 + 512].rearrange("(k p) n -> p k n", p=128)
                cx.dma("sp", lambda e: e.dma_start(out=wt[:], in_=src), writes=[wr])
                bt, br = bring.next()
                cx.dma("sp", lambda e: e.dma_start(out=bt[:], in_=ada_b_l[col0:col0 + 512].partition_broadcast(1)), writes=[br])
                pa, pra = psA

                def mv(e):
                    last = None
                    for k in range(8):
                        last = e.matmul(pa[0:1, :], lhsT=cA[:, k:k + 1], rhs=wt[:, k, :], start=(k == 0), stop=(k == 7))
                    return last
                cx.op("pe", mv, reads=[rc, wr], writes=[pra])
                rt, rr = rowring.next()
                cx.op("dve", lambda e: e.tensor_tensor(out=rt[:], in0=pa[0:1, :], in1=bt[:], op=ALU.add), reads=[pra, br], writes=[rr])
                pb, prb = psB
                cx.op("pe", lambda e: e.matmul(pb[:, :], lhsT=K.ones_f[0:1, :], rhs=rt[:], start=True, stop=True),
                      reads=[rr, K.res], writes=[prb])
                dsl = dst[:, half * 512:(half + 1) * 512]
                if mode == "copy":
                    cx.op("act", lambda e: e.copy(out=dsl, in_=pb[:, :]), reads=[prb], writes=[dres])
                else:
                    asl = aux[:, half * 512:(half + 1) * 512]
                    cx.op("dve", lambda e: e.scalar_tensor_tensor(out=dsl, in0=pb[:, :], scalar=1.0, in1=asl, op0=ALU.add, op1=ALU.mult),
                          reads=[prb, auxres], writes=[dres])
        cx.barrier()


def emit_norm_mod(cx, xin, xres, weff, sh, modres, hout, hres, small, tmask=None):
    st, sr = small.next()
    cx.op("act", lambda e: e.activation(out=hout, in_=xin, func=AF.Square, accum_out=st[:, 0:1]), reads=[xres], writes=[hres, sr])
    cx.op("dve", lambda e: e.tensor_scalar(out=st[:, 1:2], in0=st[:, 0:1], scalar1=1.0 / D, scalar2=EPS, op0=ALU.mult, op1=ALU.add),
          reads=[sr], writes=[sr])
    cx.op("act", lambda e: e.activation(out=st[:, 2:3], in_=st[:, 1:2], func=AF.Sqrt), reads=[sr], writes=[sr])
    cx.op("dve", lambda e: e.reciprocal(out=st[:, 3:4], in_=st[:, 2:3]), reads=[sr], writes=[sr])
    if tmask is not None:
        tm, tmres = tmask
        cx.op("dve", lambda e: e.tensor_tensor(out=st[:, 3:4], in0=st[:, 3:4], in1=tm, op=ALU.mult), reads=[sr, tmres], writes=[sr])
    cx.op("dve", lambda e: e.scalar_tensor_tensor(out=hout, in0=xin, scalar=st[:, 3:4], in1=weff[:], op0=ALU.mult, op1=ALU.mult),
          reads=[xres, sr, modres], writes=[hres])
    if tmask is not None:
        tm, tmres = tmask
        cx.op("dve", lambda e: e.scalar_tensor_tensor(out=hout, in0=sh[:], scalar=tm, in1=hout, op0=ALU.mult, op1=ALU.add),
              reads=[hres, modres, tmres], writes=[hres])
    else:
        cx.op("dve", lambda e: e.tensor_tensor(out=hout, in0=hout, in1=sh[:], op=ALU.add), reads=[hres, modres], writes=[hres])


def emit_moe(cx, K, X, Xres, chunks, mod, W, PS, scr):
    nc = cx.nc
    uid = "m%d_" % cx.ninst
    NS = CAP // 128
    ps_f, ps_t = PS
    weff, modres = mod["weff"]
    sh, _ = mod["sh"]
    gb, modres_g = mod["g"]
    nch = len(chunks)
    xg_res, yg_res = scr["xg_res"], scr["yg_res"]
    with ExitStack() as es0:
        def sb(name, shape, dt):
            return es0.enter_context(nc.sbuf_tensor(uid + name, shape, dt))
        cres = Res("moe_c")
        breg = cx.bounds_reg(NE * CAP - 1)
        rw = sb("rw", [128, 8, NE], F32)
        rbb = sb("rbb", [128, NE], F32)
        b1t = sb("b1t", [128, NE, 16], F32)
        b2f = sb("b2f", [NE, D], F32)
        eoff_i = sb("eoffi", [128, NE], I32)
        eoff = sb("eoff", [128, NE], F32)
        cntb = sb("cntb", [128, NE], F32)
        slot_f = sb("slotf", [128, nch, 4], F32)
        slot_i = sb("sloti", [128, nch, 4], I32)
        gate4 = sb("gate4", [128, nch, 4], F32)
        ok4 = sb("ok4", [128, nch, 4], F32)
        ex4 = sb("ex4", [128, nch, 4], F32)
        Gd = sb("Gd", [128, nch, NE], F32)
        cx.dma("sp", lambda e: [e.dma_start(out=rw[:], in_=W["rw"]),
                                e.dma_start(out=rbb[:], in_=W["rb"].partition_broadcast(128)),
                                e.dma_start(out=b1t[:], in_=W["b1t"]),
                                e.dma_start(out=b2f[:], in_=W["b2"])], writes=[cres])
        cx.op("pool", lambda e: e.iota(eoff_i[:], pattern=[[CAP, NE]], base=0, channel_multiplier=0), writes=[cres])
        cx.op("dve", lambda e: e.tensor_copy(out=eoff[:], in_=eoff_i[:]), reads=[cres], writes=[cres])
        cx.op("dve", lambda e: e.memset(cntb[:], 0.0), reads=[cres], writes=[cres])

        NW = 6
        wring = Ring(nc, uid + "w", NW, [128, 8, 512], BF16, es0)
        units = []
        for e_ in range(NE):
            for u in range(4):
                units.append(W["w1"][e_].rearrange("(k p) n -> p k n", p=128)[:, :, u * 512:(u + 1) * 512])
            for dh in range(2):
                units.append(W["w2"][e_].rearrange("(k p) n -> p k n", p=128)[:, :, dh * 512:(dh + 1) * 512])
        loaded = {}
        state = {"next": 0}

        def prefetch(upto):
            while state["next"] <= min(upto, len(units) - 1):
                u = state["next"]
                wt, wr = wring.next()
                src = units[u]
                cx.dma("pool", lambda e: e.dma_start(out=wt[:], in_=src), writes=[wr])
                loaded[u] = (wt, wr)
                state["next"] += 1

        prefetch(NW - 1)
        rres = [Res("route%d" % i) for i in range(nch)]

        with ExitStack() as es:
            hring = Ring(nc, uid + "h", 2, [128, D], F32, es)
            hbring = Ring(nc, uid + "hb", 2, [128, D], BF16, es)
            hTring = Ring(nc, uid + "hT", 2, [128, 8, 128], F32, es)
            small = Ring(nc, uid + "sm", 2, [128, 4], F32, es)
            lring = Ring(nc, uid + "lg", 2, [128, NE], F32, es)
            m8ring = Ring(nc, uid + "m8", 2, [128, 8], F32, es)
            mkring = Ring(nc, uid + "mk", 2, [128, NE], BF16, es)
            svring = Ring(nc, uid + "sv", 2, [128, NE], F32, es)
            ohring = Ring(nc, uid + "oh", 2, [128, NE], F32, es)
            jkring = Ring(nc, uid + "jk", 2, [128, NE], F32, es)
            for ci, c in enumerate(chunks):
                xin = X[:, c, :]
                ht, hr = hring.next()
                emit_norm_mod(cx, xin, Xres[c], weff, sh, modres, ht[:], hr, small)
                hb, hbr = hbring.next()
                cx.op("act", lambda e: e.copy(out=hb[:], in_=ht[:]), reads=[hr], writes=[hbr])
                hT, hTr = hTring.next()
                for half in range(2):
                    pt, ptr = ps_f[half]

                    def tr(e):
                        last = None
                        for kk in range(4):
                            k = half * 4 + kk
                            last = e.transpose(out=pt[:, kk * 128:(kk + 1) * 128], in_=ht[:, k * 128:(k + 1) * 128], identity=K.ident_f)
                        return last
                    cx.op("pe", tr, reads=[hr, K.res], writes=[ptr])
                    cx.op("act", lambda e: e.copy(out=hT[:, half * 4:half * 4 + 4, :], in_=pt[:, :].rearrange("p (k t) -> p k t", k=4)),
                          reads=[ptr], writes=[hTr])
                pl, plr = ps_f[2]

                def rt(e):
                    last = None
                    for k in range(8):
                        last = e.matmul(pl[:, 0:NE], lhsT=hT[:, k, :], rhs=rw[:, k, :], start=(k == 0), stop=(k == 7))
                    return last
                cx.op("pe", rt, reads=[hTr, cres], writes=[plr])
                lg, lgr = lring.next()
                cx.op("dve", lambda e: e.tensor_tensor(out=lg[:], in0=pl[:, 0:NE], in1=rbb[:], op=ALU.add), reads=[plr, cres], writes=[lgr])
                m8, m8r = m8ring.next()
                cx.op("dve", lambda e: e.max(out=m8[:], in_=lg[:]), reads=[lgr], writes=[m8r])
                rr = rres[ci]
                st, sr = small.next()
                cx.op("dve", lambda e: e.tensor_scalar(out=st[:, 0:1], in0=m8[:, 0:1], scalar1=-1.0, scalar2=None, op0=ALU.mult), reads=[m8r], writes=[sr])
                cx.op("act", lambda e: e.activation(out=ex4[:, ci, :], in_=m8[:, 0:4], func=AF.Exp, bias=st[:, 0:1], accum_out=st[:, 1:2]),
                      reads=[m8r, sr], writes=[rr, sr])
                cx.op("dve", lambda e: e.reciprocal(out=st[:, 2:3], in_=st[:, 1:2]), reads=[sr], writes=[sr])
                cx.op("dve", lambda e: e.tensor_scalar(out=gate4[:, ci, :], in0=ex4[:, ci, :], scalar1=st[:, 2:3], scalar2=None, op0=ALU.mult),
                      reads=[rr, sr], writes=[rr])
                mk, mkr = mkring.next()
                cx.op("dve", lambda e: e.tensor_scalar(out=mk[:], in0=lg[:], scalar1=m8[:, 3:4], scalar2=None, op0=ALU.is_ge), reads=[lgr, m8r], writes=[mkr])
                pp, ppr = ps_f[3]

                def posmm(e):
                    e.matmul(pp[:, 0:NE], lhsT=K.tris_b, rhs=mk[:], start=True, stop=True)
                    return e.matmul(pp[:, NE:2 * NE], lhsT=K.ones_b, rhs=mk[:], start=True, stop=True)
                cx.op("pe", posmm, reads=[mkr, K.res], writes=[ppr])
                sv, svr = svring.next()
                jk, jkr = jkring.next()
                cx.op("dve", lambda e: e.tensor_tensor(out=sv[:], in0=pp[:, 0:NE], in1=cntb[:], op=ALU.add), reads=[ppr, cres], writes=[svr])
                cx.op("dve", lambda e: e.tensor_tensor(out=cntb[:], in0=pp[:, NE:2 * NE], in1=cntb[:], op=ALU.add), reads=[ppr, cres], writes=[cres])
                cx.op("dve", lambda e: e.tensor_scalar(out=jk[:], in0=sv[:], scalar1=float(CAP), scalar2=1.0e7, op0=ALU.is_ge, op1=ALU.mult),
                      reads=[svr], writes=[jkr])
                cx.op("dve", lambda e: e.tensor_tensor(out=sv[:], in0=sv[:], in1=jk[:], op=ALU.add), reads=[svr, jkr], writes=[svr])
                cx.op("dve", lambda e: e.tensor_tensor(out=sv[:], in0=sv[:], in1=eoff[:], op=ALU.add), reads=[svr, cres], writes=[svr])
                cx.op("dve", lambda e: e.memset(Gd[:, ci, :], 0.0), reads=[rr], writes=[rr])
                for k in range(4):
                    oh, ohr = ohring.next()
                    cx.op("dve", lambda e: e.tensor_scalar(out=oh[:], in0=lg[:], scalar1=m8[:, k:k + 1], scalar2=None, op0=ALU.is_equal),
                          reads=[lgr, m8r], writes=[ohr])
                    jk2, jk2r = jkring.next()
                    cx.op("dve", lambda e: e.scalar_tensor_tensor(out=jk2[:], in0=oh[:], scalar=1.0, in1=sv[:], op0=ALU.mult, op1=ALU.mult,
                                                                  accum_out=slot_f[:, ci, k:k + 1]), reads=[ohr, svr, rr], writes=[jk2r, rr])
                    cx.op("dve", lambda e: e.scalar_tensor_tensor(out=Gd[:, ci, :], in0=oh[:], scalar=gate4[:, ci, k:k + 1], in1=Gd[:, ci, :],
                                                                  op0=ALU.mult, op1=ALU.add), reads=[ohr, rr], writes=[rr])
                cx.op("dve", lambda e: e.tensor_scalar(out=ok4[:, ci, :], in0=slot_f[:, ci, :], scalar1=float(NE * CAP), scalar2=None, op0=ALU.is_lt),
                      reads=[rr], writes=[rr])
                cx.op("dve", lambda e: e.tensor_tensor(out=gate4[:, ci, :], in0=gate4[:, ci, :], in1=ok4[:, ci, :], op=ALU.mult), reads=[rr], writes=[rr])
                cx.op("dve", lambda e: e.tensor_copy(out=slot_i[:, ci, :], in_=slot_f[:, ci, :]), reads=[rr], writes=[rr])
                for k in range(4):
                    cx.dma("pool", lambda e: e.indirect_dma_start(out=scr["xg"], out_offset=bass.IndirectOffsetOnAxis(ap=slot_i[:, ci, k:k + 1], axis=0),
                                                                  in_=hb[:], in_offset=None, bounds_check=breg, oob_is_err=False),
                           reads=[hbr, rr], writes=[xg_res])
            cx.barrier(extra=[xg_res.w])

        with ExitStack() as es:
            xgring = Ring(nc, uid + "xg", 2, [128, NS, D], BF16, es)
            xgTring = Ring(nc, uid + "xgT", 1, [128, 8, CAP], BF16, es)
            actring = Ring(nc, uid + "act", 1, [128, 8, CAP], BF16, es)
            tgring = Ring(nc, uid + "tg", 2, [128, CAP], F32, es)
            tsring = Ring(nc, uid + "ts", 2, [128, CAP], F32, es)
            tlring = Ring(nc, uid + "tl", 2, [128, CAP], F32, es)
            stg = Ring(nc, uid + "stg", 2, [128, D], F32, es)
            h1banks = [ps_f[0], ps_f[1], ps_f[2], ps_f[3]]
            ybanks = [ps_f[4], ps_f[5]]
            hb_i = 0
            xgs = {}

            def load_xg(e_):
                if e_ >= NE:
                    return
                xg, xgr = xgring.next()
                srcx = scr["xg"][e_ * CAP:(e_ + 1) * CAP, :].rearrange("(s p) d -> p s d", p=128)
                cx.dma("sp", lambda e: e.dma_start(out=xg[:], in_=srcx), reads=[xg_res], writes=[xgr])
                xgs[e_] = (xg, xgr)
            load_xg(0)
            for e_ in range(NE):
                load_xg(e_ + 1)
                xg, xgr = xgs.pop(e_)
                xgT, xgTr = xgTring.next()
                pt, ptr = ps_t
                for s in range(NS):
                    def trb(e):
                        last = None
                        for k in range(8):
                            last = e.transpose(out=pt[:, k * 128:(k + 1) * 128], in_=xg[:, s, k * 128:(k + 1) * 128], identity=K.ident_b)
                        return last
                    cx.op("pe", trb, reads=[xgr, K.res], writes=[ptr])
                    cx.op("act", lambda e: e.copy(out=xgT[:, :, s * 128:(s + 1) * 128], in_=pt[:, :].rearrange("p (k t) -> p k t", k=8)),
                          reads=[ptr], writes=[xgTr])
                act, actr = actring.next()
                for u in range(4):
                    wt, wr = loaded[6 * e_ + u]
                    for jj in range(2):
                        j = u * 2 + jj
                        pg, pgr = h1banks[hb_i % 4]; hb_i += 1
                        pl_, plr_ = h1banks[hb_i % 4]; hb_i += 1

                        def mmg(e):
                            last = None
                            for k in range(8):
                                last = e.matmul(pg[:, 0:CAP], lhsT=wt[:, k, jj * 256:(jj + 1) * 256:2], rhs=xgT[:, k, :], start=(k == 0), stop=(k == 7))
                            return last

                        def mml(e):
                            last = None
                            for k in range(8):
                                last = e.matmul(pl_[:, 0:CAP], lhsT=wt[:, k, jj * 256 + 1:(jj + 1) * 256:2], rhs=xgT[:, k, :], start=(k == 0), stop=(k == 7))
                            return last
                        cx.op("pe", mmg, reads=[wr, xgTr], writes=[pgr])
                        cx.op("pe", mml, reads=[wr, xgTr], writes=[plr_])
                        tg, tgr = tgring.next()
                        ts, tsr = tsring.next()
                        tl, tlr = tlring.next()
                        cx.op("dve", lambda e: e.tensor_scalar(out=tg[:], in0=pg[:, 0:CAP], scalar1=b1t[:, e_, j:j + 1], scalar2=7.0, op0=ALU.add, op1=ALU.min),
                              reads=[pgr, cres], writes=[tgr])
                        cx.op("act", lambda e: e.activation(out=ts[:], in_=tg[:], func=AF.Sigmoid, scale=1.702), reads=[tgr], writes=[tsr])
                        cx.op("dve", lambda e: e.tensor_scalar(out=tl[:], in0=pl_[:, 0:CAP], scalar1=b1t[:, e_, 8 + j:9 + j], scalar2=7.0, op0=ALU.add, op1=ALU.min),
                              reads=[plr_, cres], writes=[tlr])
                        cx.op("dve", lambda e: e.tensor_scalar(out=tl[:], in0=tl[:], scalar1=-7.0, scalar2=1.0, op0=ALU.max, op1=ALU.add),
                              reads=[tlr], writes=[tlr])
                        cx.op("dve", lambda e: e.tensor_tensor(out=tg[:], in0=tg[:], in1=ts[:], op=ALU.mult), reads=[tgr, tsr], writes=[tgr])
                        cx.op("dve", lambda e: e.tensor_tensor(out=act[:, j, :], in0=tg[:], in1=tl[:], op=ALU.mult), reads=[tgr, tlr], writes=[actr])
                    prefetch(6 * e_ + u + NW)
                for s in range(NS):
                    so, sor = stg.next()
                    for dh in range(2):
                        wt2, wr2 = loaded[6 * e_ + 4 + dh]
                        py, pyr = ybanks[dh]

                        def mmy(e):
                            last = None
                            for k in range(8):
                                last = e.matmul(py[:, :], lhsT=act[:, k, s * 128:(s + 1) * 128], rhs=wt2[:, k, :], start=(k == 0), stop=(k == 7))
                            return last
                        cx.op("pe", mmy, reads=[actr, wr2], writes=[pyr])
                        cx.op("act", lambda e: e.copy(out=so[:, dh * 512:(dh + 1) * 512], in_=py[:, :]), reads=[pyr], writes=[sor])
                    r0 = e_ * CAP + s * 128
                    cx.dma("sp", lambda e: e.dma_start(out=scr["yg"][r0:r0 + 128, :], in_=so[:]), reads=[sor], writes=[yg_res])
                prefetch(6 * e_ + 5 + NW)
            cx.barrier(extra=[yg_res.w])

        with ExitStack() as es:
            ykring = Ring(nc, uid + "yk", 4, [128, D], F32, es)
            accring = Ring(nc, uid + "acc", 2, [128, D], F32, es)
            gtring = Ring(nc, uid + "gt", 2, [NE, 128], F32, es)
            for ci, c in enumerate(chunks):
                rr = rres[ci]
                yks = []
                for k in range(4):
                    yk, ykr = ykring.next()
                    cx.dma("pool", lambda e: e.indirect_dma_start(out=yk[:], out_offset=None, in_=scr["yg"],
                                                                  in_offset=bass.IndirectOffsetOnAxis(ap=slot_i[:, ci, k:k + 1], axis=0),
                                                                  bounds_check=breg, oob_is_err=False),
                           reads=[yg_res, rr], writes=[ykr])
                    yks.append((yk, ykr))
                pg_, pgr_ = ps_f[6]
                cx.op("pe", lambda e: e.transpose(out=pg_[0:NE, 0:128], in_=Gd[:, ci, :], identity=K.ident_f), reads=[rr, K.res], writes=[pgr_])
                gt, gtr = gtring.next()
                cx.op("act", lambda e: e.copy(out=gt[:], in_=pg_[0:NE, 0:128]), reads=[pgr_], writes=[gtr])
                acc, accr = accring.next()
                for dh in range(2):
                    pb_, pbr_ = ps_f[dh]
                    cx.op("pe", lambda e: e.matmul(pb_[:, :], lhsT=gt[:], rhs=b2f[:, dh * 512:(dh + 1) * 512], start=True, stop=True),
                          reads=[gtr, cres], writes=[pbr_])
                    cx.op("act", lambda e: e.copy(out=acc[:, dh * 512:(dh + 1) * 512], in_=pb_[:, :]), reads=[pbr_], writes=[accr])
                for k in range(4):
                    yk, ykr = yks[k]
                    cx.op("dve", lambda e: e.scalar_tensor_tensor(out=acc[:], in0=yk[:], scalar=gate4[:, ci, k:k + 1], in1=acc[:], op0=ALU.mult, op1=ALU.add),
                          reads=[ykr, rr, accr], writes=[accr])
                cx.op("dve", lambda e: e.tensor_tensor(out=acc[:], in0=acc[:], in1=gb[:], op=ALU.mult), reads=[accr, modres_g], writes=[accr])
                cx.op("dve", lambda e: e.tensor_tensor(out=X[:, c, :], in0=X[:, c, :], in1=acc[:], op=ALU.add), reads=[accr, Xres[c]], writes=[Xres[c]])
            cx.barrier()


NPRE = 47
SLABW = 648


DBG_STOP = 0


def emit_mamba(cx, K, X, Xres, mod, W, PSB, tmask, tmres, NPRE=NPRE, NMAIN=17):
    nc = cx.nc
    uid = "mb%d_" % cx.ninst
    weff, modres = mod["weff"]
    sh, _ = mod["sh"]
    gb, modres_g = mod["g"]
    NCHT = NPRE + NMAIN
    groups = [list(range(i, i + 4)) for i in range(0, NCHT, 4)]
    with ExitStack() as es:
        def sb(name, shape, dt):
            return es.enter_context(nc.sbuf_tensor(uid + name, shape, dt))
        cres = Res("mb_c")
        cw = sb("cw", [128, 24, 4], F32)
        cb = sb("cb", [128, 24], F32)
        dtb = sb("dtb", [128, 32], F32)
        Ab = sb("Ab", [128, 32], F32)
        Db = sb("Db", [128, 32], F32)
        cvh = sb("cvh", [128, 24, 3], F32)
        S = sb("S", [128, 4, 512], F32)
        Sb = sb("Sb", [128, 4, 512], BF16)
        cx.dma("sp", lambda e: [e.dma_start(out=cw[:], in_=W["cw"]), e.dma_start(out=cb[:], in_=W["cb"]),
                                e.dma_start(out=dtb[:], in_=W["dtb"].partition_broadcast(128)),
                                e.dma_start(out=Ab[:], in_=W["alog"].partition_broadcast(128)),
                                e.dma_start(out=Db[:], in_=W["dsk"].partition_broadcast(128))], writes=[cres])
        cx.op("act", lambda e: e.activation(out=Ab[:], in_=Ab[:], func=AF.Exp), reads=[cres], writes=[cres])
        cx.op("dve", lambda e: e.tensor_scalar(out=Ab[:], in0=Ab[:], scalar1=-1.0, scalar2=None, op0=ALU.mult), reads=[cres], writes=[cres])
        cx.op("dve", lambda e: e.memset(cvh[:], 0.0), reads=[cres], writes=[cres])
        Sres = [Res("S%d" % g) for g in range(4)]
        Sbres = [Res("Sb%d" % g) for g in range(4)]
        for g in range(4):
            cx.op("dve", lambda e: e.memset(S[:, g, :], 0.0), writes=[Sres[g]])
            cx.op("dve", lambda e: e.memset(Sb[:, g, :], 0.0), writes=[Sbres[g]])
        cvres = [Res("cv%d" % i) for i in range(24)]
        for r_ in cvres:
            r_.w = cres.w

        slabs = Ring(nc, uid + "slab", 3, [128, 8, SLABW], BF16, es)
        owring = Ring(nc, uid + "ow", 2, [128, 4, D], BF16, es)
        ngring = Ring(nc, uid + "ng", 2, [128, 512], F32, es)
        hTring = Ring(nc, uid + "hT", 1, [128, 8, 512], BF16, es)
        hring = Ring(nc, uid + "h", 1, [128, D], F32, es)
        hbring = Ring(nc, uid + "hb", 1, [128, D], BF16, es)
        xpring = Ring(nc, uid + "xp", 2, [128, D], F32, es)
        small = Ring(nc, uid + "sm", 2, [128, 4], F32, es)
        pbring = Ring(nc, uid + "pb", 2, [128, 515], F32, es)
        acring = Ring(nc, uid + "ac", 2, [128, 512], F32, es)
        xsT = sb("xsT", [128, 4, 512], BF16)
        BT = sb("BT", [128, 512], BF16)
        CT = sb("CT", [128, 512], BF16)
        ftres = [Res("ft%d" % i) for i in range(6)]
        dtring = Ring(nc, uid + "dt", 2, [128, 64], F32, es)
        Xbring = Ring(nc, uid + "Xb", 2, [128, 512], BF16, es)
        Xdring = Ring(nc, uid + "Xd", 1, [128, 512], BF16, es)
        xtring = Ring(nc, uid + "xt", 1, [128, 512], BF16, es)
        Btring = Ring(nc, uid + "Bt", 2, [128, 128], BF16, es)
        mcbring = Ring(nc, uid + "mcb", 2, [128, 128], F32, es)
        dabring = Ring(nc, uid + "dab", 2, [128, 128], F32, es)
        ehring = Ring(nc, uid + "eh", 2, [128, 128], F32, es)
        mtring = Ring(nc, uid + "mt", 2, [128, 128], BF16, es)
        yring = Ring(nc, uid + "y", 1, [128, 512], F32, es)
        y2ring = Ring(nc, uid + "y2", 1, [128, 512], F32, es)
        zsring = Ring(nc, uid + "zs", 1, [128, 512], F32, es)
        ynring = Ring(nc, uid + "yn", 1, [128, 512], BF16, es)
        ynTring = Ring(nc, uid + "ynT", 2, [128, 4, 128], BF16, es)
        oring = Ring(nc, uid + "o", 1, [128, 512], F32, es)
        P = PSB
        tbank = (P[3][0][:, :].bitcast(BF16), P[3][1])

        try:
         for cs in groups:
             mains = [c >= NPRE for c in cs]
             full = any(mains)
             hT, hTr = hTring.next()
             for j, c in enumerate(cs):
                 if mains[j]:
                     ci = c - NPRE
                     xin, xres = X[:, ci, :], Xres[ci]
                 else:
                     xp, xpr = xpring.next()
                     cx.dma("sp", lambda e: e.dma_start(out=xp[:], in_=W["xpre"][c * 128:(c + 1) * 128, :]), writes=[xpr])
                     xin, xres = xp[:], xpr
                 ht, hr = hring.next()
                 emit_norm_mod(cx, xin, xres, weff, sh, modres, ht[:], hr, small, tmask=(tmask[:, c:c + 1], tmres))
                 hb, hbr = hbring.next()
                 cx.op("act", lambda e: e.copy(out=hb[:], in_=ht[:]), reads=[hr], writes=[hbr])
                 tb, tbr = tbank

                 def tr(e):
                     last = None
                     for k in range(8):
                         last = e.transpose(out=tb[:, k * 128:(k + 1) * 128], in_=hb[:, k * 128:(k + 1) * 128], identity=K.ident_b)
                     return last
                 cx.op("pe", tr, reads=[hbr, K.res], writes=[tbr])
                 cx.op("act", lambda e: e.copy(out=hT[:, :, j * 128:(j + 1) * 128], in_=tb.rearrange("p (k t) -> p k t", k=8)),
                       reads=[tbr], writes=[hTr])
             if DBG_STOP == 1:
                 raise StopEmit()
             for g in range(4):
                 sA, sAr = slabs.next()
                 cx.dma("pool", lambda e: e.dma_start(out=sA[:], in_=W["inA"][g].rearrange("(k p) n -> p k n", p=128)), writes=[sAr])
                 if full:
                     sB, sBr = slabs.next()
                     cx.dma("pool", lambda e: e.dma_start(out=sB[:], in_=W["inB"][g].rearrange("(k p) n -> p k n", p=128)), writes=[sBr])
                     ow, owr = owring.next()
                     cx.dma("pool", lambda e: e.dma_start(out=ow[:], in_=W["outw"][g * 512:(g + 1) * 512, :].rearrange("(k p) n -> p k n", p=128)), writes=[owr])
                     ng, ngr = ngring.next()
                     cx.dma("sp", lambda e: e.dma_start(out=ng[:], in_=W["normg"][g * 512:(g + 1) * 512].partition_broadcast(128)), writes=[ngr])
                 ntile = 6 if full else 5
                 for t in range(ntile):
                     if t < 5:
                         sl, slr, c0 = sA, sAr, t * 128
                     else:
                         sl, slr, c0 = sB, sBr, 0
                     pf, pfr = P[t % 2]

                     def mmf(e):
                         last = None
                         for k in range(8):
                             last = e.matmul(pf[:, :], lhsT=sl[:, k, c0:c0 + 128], rhs=hT[:, k, :], start=(k == 0), stop=(k == 7))
                         return last
                     cx.op("pe", mmf, reads=[slr, hTr], writes=[pfr])
                     ti = g * 6 + t
                     pb, pbr = pbring.next()
                     cx.op("act", lambda e: e.copy(out=pb[:, 3:515], in_=pf[:, :]), reads=[pfr], writes=[pbr])
                     cx.op("dve", lambda e: e.tensor_copy(out=pb[:, 0:3], in_=cvh[:, ti, :]), reads=[cvres[ti], pbr], writes=[pbr])
                     cx.op("dve", lambda e: e.tensor_copy(out=cvh[:, ti, :], in_=pb[:, 512:515]), reads=[pbr], writes=[cvres[ti]])
                     ac, acr = acring.next()
                     cx.op("dve", lambda e: e.tensor_scalar(out=ac[:], in0=pb[:, 0:512], scalar1=cw[:, ti, 0:1], scalar2=None, op0=ALU.mult),
                           reads=[pbr, cres], writes=[acr])
                     for tap in range(1, 4):
                         cx.op("dve", lambda e: e.scalar_tensor_tensor(out=ac[:], in0=pb[:, tap:tap + 512], scalar=cw[:, ti, tap:tap + 1], in1=ac[:],
                                                                       op0=ALU.mult, op1=ALU.add), reads=[pbr, cres, acr], writes=[acr])
                     dst = xsT[:, t, :] if t < 4 else (BT[:] if t == 4 else CT[:])
                     cx.op("act", lambda e: e.activation(out=dst, in_=ac[:], func=AF.Silu, bias=cb[:, ti:ti + 1]), reads=[acr, cres], writes=[ftres[t]])
                 if DBG_STOP == 2:
                     raise StopEmit()
                 for j, c in enumerate(cs):
                     tsl = slice(j * 128, (j + 1) * 128)
                     dt_, dtr = dtring.next()
                     pd, pdr = P[2]

                     def mmdt(e):
                         last = None
                         for k in range(8):
                             last = e.matmul(pd[:, 0:8], lhsT=hT[:, k, tsl], rhs=sA[:, k, 640:648], start=(k == 0), stop=(k == 7))
                         return last
                     cx.op("pe", mmdt, reads=[hTr, sAr], writes=[pdr])
                     cx.op("dve", lambda e: e.tensor_tensor(out=dt_[:, 56:64], in0=pd[:, 0:8], in1=dtb[:, g * 8:(g + 1) * 8], op=ALU.add),
                           reads=[pdr, cres], writes=[dtr])
                     cx.op("act", lambda e: e.activation(out=dt_[:, 56:64], in_=dt_[:, 56:64], func=AF.Exp), reads=[dtr], writes=[dtr])
                     cx.op("act", lambda e: e.activation(out=dt_[:, 0:8], in_=dt_[:, 56:64], func=AF.Ln, bias=1.0), reads=[dtr], writes=[dtr])
                     cx.op("dve", lambda e: e.tensor_scalar(out=dt_[:, 0:8], in0=dt_[:, 0:8], scalar1=tmask[:, c:c + 1], scalar2=None, op0=ALU.mult),
                           reads=[dtr, tmres], writes=[dtr])
                     cx.op("dve", lambda e: e.tensor_tensor(out=dt_[:, 8:16], in0=dt_[:, 0:8], in1=Ab[:, g * 8:(g + 1) * 8], op=ALU.mult),
                           reads=[dtr, cres], writes=[dtr])

                     def mmcum(e):
                         e.matmul(pd[:, 16:24], lhsT=K.trii_f, rhs=dt_[:, 8:16], start=True, stop=True)
                         return e.matmul(pd[:, 24:32], lhsT=K.ones_f, rhs=dt_[:, 8:16], start=True, stop=True)
                     cx.op("pe", mmcum, reads=[dtr, K.res], writes=[pdr])
                     cx.op("dve", lambda e: e.tensor_copy(out=dt_[:, 16:24], in_=pd[:, 16:24]), reads=[pdr], writes=[dtr])
                     cx.op("dve", lambda e: e.tensor_scalar(out=dt_[:, 24:32], in0=pd[:, 16:24], scalar1=-1.0, scalar2=None, op0=ALU.mult), reads=[pdr], writes=[dtr])
                     cx.op("dve", lambda e: e.tensor_tensor(out=dt_[:, 56:64], in0=pd[:, 24:32], in1=dt_[:, 16:24], op=ALU.subtract), reads=[pdr, dtr], writes=[dtr])
                     cx.op("act", lambda e: e.activation(out=dt_[:, 32:40], in_=dt_[:, 56:64], func=AF.Exp), reads=[dtr], writes=[dtr])
                     cx.op("act", lambda e: e.activation(out=dt_[:, 40:48], in_=dt_[:, 16:24], func=AF.Exp), reads=[dtr], writes=[dtr])
                     cx.op("dve", lambda e: e.tensor_copy(out=dt_[:, 48:56], in_=pd[:, 24:32]), reads=[pdr, dtr], writes=[dtr])
                     cx.op("act", lambda e: e.activation(out=dt_[:, 48:56], in_=dt_[:, 48:56], func=AF.Exp), reads=[dtr], writes=[dtr])
                     if DBG_STOP == 3:
                         raise StopEmit()
                     tb, tbr = tbank

                     def trx(e):
                         last = None
                         for t in range(4):
                             last = e.transpose(out=tb[:, t * 128:(t + 1) * 128], in_=xsT[:, t, tsl], identity=K.ident_b)
                         last = e.transpose(out=tb[:, 512:640], in_=BT[:, tsl], identity=K.ident_b)
                         return last
                     cx.op("pe", trx, reads=[ftres[0], ftres[1], ftres[2], ftres[3], ftres[4], K.res], writes=[tbr])
                     Xb, Xbr = Xbring.next()
                     Xd, Xdr = Xdring.next()
                     Bt, Btr = Btring.next()
                     cx.op("dve", lambda e: e.tensor_tensor(out=Xb[:, :].rearrange("p (h q) -> p h q", h=8), in0=tb[:, 0:512].rearrange("p (h q) -> p h q", h=8),
                                                            in1=dt_[:, 0:8].unsqueeze(2).to_broadcast([128, 8, 64]), op=ALU.mult),
                           reads=[tbr, dtr], writes=[Xbr])
                     cx.op("dve", lambda e: e.tensor_tensor(out=Xd[:, :].rearrange("p (h q) -> p h q", h=8), in0=Xb[:, :].rearrange("p (h q) -> p h q", h=8),
                                                            in1=dt_[:, 32:40].unsqueeze(2).to_broadcast([128, 8, 64]), op=ALU.mult),
                           reads=[Xbr, dtr], writes=[Xdr])
                     cx.op("dve", lambda e: e.tensor_copy(out=Bt[:], in_=tb[:, 512:640]), reads=[tbr], writes=[Btr])
                     if DBG_STOP == 4:
                         raise StopEmit()
                     if mains[j]:
                         ci = c - NPRE
                         xt, xtr = xtring.next()
                         cx.op("dve", lambda e: e.tensor_copy(out=xt[:], in_=tb[:, 0:512]), reads=[tbr], writes=[xtr])
                         pc, pcr = P[2]
                         cx.op("pe", lambda e: e.matmul(pc[:, 128:256], lhsT=BT[:, tsl], rhs=CT[:, tsl], start=True, stop=True),
                               reads=[ftres[4], ftres[5]], writes=[pcr])
                         mcb, mcbr = mcbring.next()
                         cx.op("dve", lambda e: e.tensor_tensor(out=mcb[:], in0=pc[:, 128:256], in1=K.trii_f, op=ALU.mult), reads=[pcr, K.res], writes=[mcbr])
                         pyd, pydr = P[6]
                         for h in range(8):
                             pL, pLr = P[4 + (h // 4) % 2]
                             dab, dabr = dabring.next()
                             cx.op("dve", lambda e: e.tensor_copy(out=dab[:], in_=dt_[:, 8 + h:9 + h].to_broadcast([128, 128])), reads=[dtr], writes=[dabr])
                             lsl = slice((h % 4) * 128, (h % 4 + 1) * 128)
                             cx.op("pe", lambda e: e.matmul(pL[:, lsl], lhsT=dab[:], rhs=K.trii_f, start=True, stop=True), reads=[dabr, K.res], writes=[pLr])
                             eh, ehr = ehring.next()
                             cx.op("dve", lambda e: e.tensor_scalar(out=eh[:], in0=pL[:, lsl], scalar1=dt_[:, 24 + h:25 + h], scalar2=0.0, op0=ALU.add, op1=ALU.min),
                                   reads=[pLr, dtr], writes=[ehr])
                             cx.op("act", lambda e: e.activation(out=eh[:], in_=eh[:], func=AF.Exp), reads=[ehr], writes=[ehr])
                             mt, mtr = mtring.next()
                             cx.op("dve", lambda e: e.tensor_tensor(out=mt[:], in0=eh[:], in1=mcb[:], op=ALU.mult), reads=[ehr, mcbr], writes=[mtr])
                             cx.op("pe", lambda e: e.matmul(pyd[:, h * 64:(h + 1) * 64], lhsT=mt[:], rhs=Xb[:, h * 64:(h + 1) * 64], start=True, stop=True),
                                   reads=[mtr, Xbr], writes=[pydr])
                         if DBG_STOP == 5:
                             raise StopEmit()
                         pyo, pyor = P[7]
                         cx.op("pe", lambda e: e.matmul(pyo[:, :], lhsT=CT[:, tsl], rhs=Sb[:, g, :], start=True, stop=True), reads=[ftres[5], Sbres[g]], writes=[pyor])
                         y, yr = yring.next()
                         cx.op("dve", lambda e: e.tensor_tensor(out=y[:, :].rearrange("p (h q) -> p h q", h=8), in0=pyo[:, :].rearrange("p (h q) -> p h q", h=8),
                                                                in1=dt_[:, 40:48].unsqueeze(2).to_broadcast([128, 8, 64]), op=ALU.mult),
                               reads=[pyor, dtr], writes=[yr])
                         y2, y2r = y2ring.next()
                         cx.op("dve", lambda e: e.tensor_tensor(out=y2[:, :].rearrange("p (h q) -> p h q", h=8), in0=xt[:, :].rearrange("p (h q) -> p h q", h=8),
                                                                in1=Db[:, g * 8:(g + 1) * 8].unsqueeze(2).to_broadcast([128, 8, 64]), op=ALU.mult),
                               reads=[xtr, cres], writes=[y2r])
                         cx.op("dve", lambda e: e.tensor_tensor(out=y[:], in0=y[:], in1=y2[:], op=ALU.add), reads=[yr, y2r], writes=[yr])
                         cx.op("dve", lambda e: e.tensor_tensor(out=y[:], in0=pyd[:, :], in1=y[:], op=ALU.add), reads=[yr, pydr], writes=[yr])
                         pz, pzr = P[7]

                         def mmz(e):
                             last = None
                             for k in range(8):
                                 last = e.matmul(pz[:, :], lhsT=hT[:, k, tsl], rhs=sB[:, k, 128:640], start=(k == 0), stop=(k == 7))
                             return last
                         cx.op("pe", mmz, reads=[hTr, sBr], writes=[pzr])
                         zs, zsr = zsring.next()
                         cx.op("act", lambda e: e.activation(out=zs[:], in_=pz[:, :], func=AF.Silu), reads=[pzr], writes=[zsr])
                         cx.op("dve", lambda e: e.tensor_tensor(out=y[:], in0=y[:], in1=zs[:], op=ALU.mult), reads=[yr, zsr], writes=[yr])
                         if DBG_STOP == 6:
                             raise StopEmit()
                         st, sr = small.next()
                         cx.op("act", lambda e: e.activation(out=y2[:], in_=y[:], func=AF.Square, accum_out=st[:, 0:1]), reads=[yr], writes=[y2r, sr])
                         cx.op("dve", lambda e: e.tensor_scalar(out=st[:, 1:2], in0=st[:, 0:1], scalar1=1.0 / 512, scalar2=EPS, op0=ALU.mult, op1=ALU.add),
                               reads=[sr], writes=[sr])
                         cx.op("act", lambda e: e.activation(out=st[:, 2:3], in_=st[:, 1:2], func=AF.Sqrt), reads=[sr], writes=[sr])
                         cx.op("dve", lambda e: e.reciprocal(out=st[:, 3:4], in_=st[:, 2:3]), reads=[sr], writes=[sr])
                         yn, ynr = ynring.next()
                         cx.op("dve", lambda e: e.scalar_tensor_tensor(out=yn[:], in0=y[:], scalar=st[:, 3:4], in1=ng[:], op0=ALU.mult, op1=ALU.mult),
                               reads=[yr, sr, ngr], writes=[ynr])

                         def try_(e):
                             last = None
                             for t in range(4):
                                 last = e.transpose(out=tb[:, t * 128:(t + 1) * 128], in_=yn[:, t * 128:(t + 1) * 128], identity=K.ident_b)
                             return last
                         cx.op("pe", try_, reads=[ynr, K.res], writes=[tbr])
                         ynT, ynTr = ynTring.next()
                         cx.op("act", lambda e: e.copy(out=ynT[:], in_=tb[:, 0:512].rearrange("p (k t) -> p k t", k=4)), reads=[tbr], writes=[ynTr])
                         for half in range(2):
                             po, por = P[half]

                             def mmo(e):
                                 last = None
                                 for k in range(4):
                                     last = e.matmul(po[:, :], lhsT=ynT[:, k, :], rhs=ow[:, k, half * 512:(half + 1) * 512], start=(k == 0), stop=(k == 3))
                                 return last
                             cx.op("pe", mmo, reads=[ynTr, owr], writes=[por])
                             o, orr = oring.next()
                             hs = slice(half * 512, (half + 1) * 512)
                             cx.op("dve", lambda e: e.tensor_tensor(out=o[:], in0=po[:, :], in1=gb[:, hs], op=ALU.mult), reads=[por, modres_g], writes=[orr])
                             cx.op("dve", lambda e: e.tensor_tensor(out=X[:, ci, hs], in0=X[:, ci, hs], in1=o[:], op=ALU.add), reads=[orr, Xres[ci]], writes=[Xres[ci]])
                     psn, psnr = P[6]
                     cx.op("pe", lambda e: e.matmul(psn[:, :], lhsT=Bt[:], rhs=Xd[:], start=True, stop=True), reads=[Btr, Xdr], writes=[psnr])
                     cx.op("dve", lambda e: e.tensor_tensor(out=S[:, g, :].rearrange("p (h q) -> p h q", h=8), in0=S[:, g, :].rearrange("p (h q) -> p h q", h=8),
                                                            in1=dt_[:, 48:56].unsqueeze(2).to_broadcast([128, 8, 64]), op=ALU.mult),
                           reads=[dtr, Sres[g]], writes=[Sres[g]])
                     cx.op("dve", lambda e: e.tensor_tensor(out=S[:, g, :], in0=psn[:, :], in1=S[:, g, :], op=ALU.add), reads=[psnr, Sres[g]], writes=[Sres[g]])
                     cx.op("act", lambda e: e.copy(out=Sb[:, g, :], in_=S[:, g, :]), reads=[Sres[g]], writes=[Sbres[g]])
        except StopEmit:
            pass
        cx.barrier()


def host_mamba_weights(in_w, conv_w, conv_b):
    inA = np.zeros((4, D, SLABW), np.float32)
    inB = np.zeros((4, D, SLABW), np.float32)
    cw = np.zeros((128, 24, 4), np.float32)
    cb = np.zeros((128, 24), np.float32)
    for g in range(4):
        xc = slice(2048 + g * 512, 2048 + (g + 1) * 512)
        bc = slice(4096 + g * 128, 4096 + (g + 1) * 128)
        cc = slice(4608 + g * 128, 4608 + (g + 1) * 128)
        inA[g, :, 0:512] = in_w[:, xc]
        inA[g, :, 512:640] = in_w[:, bc]
        inA[g, :, 640:648] = in_w[:, 5120 + g * 8:5120 + (g + 1) * 8]
        inB[g, :, 0:128] = in_w[:, cc]
        inB[g, :, 128:640] = in_w[:, g * 512:(g + 1) * 512]
        chans = [np.arange(g * 512 + t * 128, g * 512 + (t + 1) * 128) for t in range(4)]
        chans.append(np.arange(2048 + g * 128, 2048 + (g + 1) * 128))
        chans.append(np.arange(2560 + g * 128, 2560 + (g + 1) * 128))
        for t in range(6):
            cw[:, g * 6 + t, :] = conv_w[:, chans[t]].T
            cb[:, g * 6 + t] = conv_b[chans[t]]
    return dict(inA=inA, inB=inB, cw=cw, cb=cb)


def host_core_x(xb, k):
    s0 = k * 2048
    x17 = np.zeros((17 * 128, D), np.float32)
    tm = np.zeros((128, 64), np.float32)
    x17[128:] = xb[s0:s0 + 2048]
    tm[:, 48:] = 1.0
    xpre = np.zeros((NPRE * 128, D), np.float32)
    if k > 0:
        x17[:128] = xb[s0 - 128:s0]
        tm[:, 47] = 1.0
        npre = 16 * k - 1
        xpre[(NPRE - npre) * 128:] = xb[:s0 - 128]
        tm[:, NPRE - npre:NPRE] = 1.0
    return dict(x=x17, xpre=xpre, tmask=tm)


def t5_bucket_table():
    import math
    d = np.arange(128)
    dd = np.maximum(d, 1).astype(np.float32)
    large = 16 + (np.log(dd / np.float32(16)) / np.float32(math.log(128 / 16)) * np.float32(16)).astype(np.int32)
    large = np.minimum(large, 31)
    return np.where(d < 16, d, large).astype(np.int64)


def emit_attn(cx, K, X, Xres, mod, W, PSB, tmask, tmres, rscr, NB=17):
    nc = cx.nc
    uid = "at%d_" % cx.ninst
    weff, modres = mod["weff"]
    sh, _ = mod["sh"]
    gb, modres_g = mod["g"]
    P = PSB
    NEG = -30000.0
    with ExitStack() as es:
        def sb(name, shape, dt):
            return es.enter_context(nc.sbuf_tensor(uid + name, shape, dt))
        cres = Res("at_c")
        qkvw = sb("qkvw", [128, 8, 1536], BF16)
        outw = sb("outw", [128, 8, D], BF16)
        qgb = sb("qgb", [128, 64], F32)
        kgb = sb("kgb", [128, 64], F32)
        snk = sb("snk", [128, 16], F32)
        hneg = sb("hneg", [128, 1], F32)
        bias = sb("bias", [128, 16, 256], F32)
        KT = sb("KT", [128, NB, 4, 128], BF16)
        Vall = sb("Vall", [128, NB, 256], BF16)
        wres = Res("at_w")
        cx.dma("pool", lambda e: [e.dma_start(out=qkvw[:], in_=W["qkvw"].rearrange("(k p) n -> p k n", p=128)),
                                  e.dma_start(out=outw[:], in_=W["outw"].rearrange("(k p) n -> p k n", p=128))], writes=[wres])
        cx.dma("sp", lambda e: [e.dma_start(out=qgb[:], in_=W["qg"].partition_broadcast(128)),
                                e.dma_start(out=kgb[:], in_=W["kg"].partition_broadcast(128)),
                                e.dma_start(out=snk[:], in_=W["sinks"].partition_broadcast(128))], writes=[cres])
        cx.op("dve", lambda e: e.tensor_scalar(out=qgb[:], in0=qgb[:], scalar1=0.125, scalar2=None, op0=ALU.mult), reads=[cres], writes=[cres])
        cx.op("dve", lambda e: e.tensor_scalar(out=hneg[:], in0=tmask[:, NPRE:NPRE + 1], scalar1=-1.0, scalar2=-NEG, op0=ALU.add, op1=ALU.mult),
              reads=[tmres, cres], writes=[cres])
        with ExitStack() as es2:
            Rsb = es2.enter_context(nc.sbuf_tensor(uid + "Rsb", [16, 384], F32))
            rres = Res("Rsb")
            cx.op("dve", lambda e: e.memset(Rsb[:], NEG), writes=[rres])
            relb = es2.enter_context(nc.sbuf_tensor(uid + "relb", [32, 16], F32))
            ohb = es2.enter_context(nc.sbuf_tensor(uid + "ohb", [32, 128], F32))
            lres = Res("relb")
            cx.dma("sp", lambda e: [e.dma_start(out=relb[:], in_=W["relb"]), e.dma_start(out=ohb[:], in_=W["ohb"])], writes=[lres])
            pbv, pbvr = P[2]
            cx.op("pe", lambda e: e.matmul(pbv[0:16, 0:128], lhsT=relb[:], rhs=ohb[:], start=True, stop=True), reads=[lres], writes=[pbvr])
            cx.op("dve", lambda e: e.tensor_copy(out=Rsb[:, 127:255], in_=pbv[0:16, 0:128]), reads=[pbvr, rres], writes=[rres])
            dres = Res("rscr")
            cx.dma("sp", lambda e: e.dma_start(out=rscr, in_=Rsb[:]), reads=[rres], writes=[dres])
            bres = Res("bias")

            brev = Ring(nc, uid + "brev", 2, [128, 256], F32, es2)
            for h in range(16):
                bv_, bvr = brev.next()
                src = bass.AP(tensor=rscr.tensor, offset=h * 384, ap=[[1, 128], [1, 256]])
                cx.dma("sp", lambda e: e.dma_start(out=bv_[:], in_=src), reads=[dres], writes=[bvr])
                fwd = bv_[:, 0:256]
                rev = bass.AP(tensor=fwd.tensor, offset=fwd.offset + 255, ap=[[fwd.ap[0][0], 128], [-1, 256]])
                cx.op("dve", lambda e: e.tensor_copy(out=bias[:, h, :], in_=rev), reads=[bvr], writes=[bres])
            cx.barrier()

        hring = Ring(nc, uid + "h", 1, [128, D], F32, es)
        hbring = Ring(nc, uid + "hb", 1, [128, D], BF16, es)
        hTring = Ring(nc, uid + "hT", 1, [128, 8, 128], BF16, es)
        small = Ring(nc, uid + "sm", 2, [128, 4], F32, es)
        kvring = Ring(nc, uid + "kv", 1, [128, 512], F32, es)
        qrring = Ring(nc, uid + "qr", 1, [128, D], F32, es)
        sqring = Ring(nc, uid + "sq", 1, [128, D], F32, es)
        rsring = Ring(nc, uid + "rs", 2, [128, 32], F32, es)
        kdring = Ring(nc, uid + "kd", 1, [128, 4, 2, 64], BF16, es)
        qnring = Ring(nc, uid + "qn", 1, [128, D], BF16, es)
        qTring = Ring(nc, uid + "qT", 1, [128, 8, 128], BF16, es)
        Sring = Ring(nc, uid + "S", 2, [128, 256], F32, es)
        pring = Ring(nc, uid + "p", 2, [128, 256], BF16, es)
        pTring = Ring(nc, uid + "pT", 2, [128, 2, 128], BF16, es)
        stat = Ring(nc, uid + "st", 4, [128, 8], F32, es)
        aoring = Ring(nc, uid + "ao", 1, [128, D], BF16, es)
        aoTring = Ring(nc, uid + "aoT", 1, [128, 8, 128], BF16, es)
        oring = Ring(nc, uid + "o", 2, [128, 512], F32, es)
        tbank = (P[3][0][:, :].bitcast(BF16), P[3][1])
        kres = [Res("KT%d" % i) for i in range(NB)]
        vres = [Res("V%d" % i) for i in range(NB)]

        for ci in range(NB):
            ht, hr = hring.next()
            emit_norm_mod(cx, X[:, ci, :], Xres[ci], weff, sh, modres, ht[:], hr, small)
            hb, hbr = hbring.next()
            cx.op("act", lambda e: e.copy(out=hb[:], in_=ht[:]), reads=[hr], writes=[hbr])
            hT, hTr = hTring.next()
            tb, tbr = tbank

            def tr(e):
                last = None
                for k in range(8):
                    last = e.transpose(out=tb[:, k * 128:(k + 1) * 128], in_=hb[:, k * 128:(k + 1) * 128], identity=K.ident_b)
                return last
            cx.op("pe", tr, reads=[hbr, K.res], writes=[tbr])
            cx.op("act", lambda e: e.copy(out=hT[:], in_=tb.rearrange("p (k t) -> p k t", k=8)), reads=[tbr], writes=[hTr])
            pk, pkr = P[2]

            def mmkv(e):
                last = None
                for k in range(8):
                    last = e.matmul(pk[:, :], lhsT=hT[:, k, :], rhs=qkvw[:, k, 1024:1536], start=(k == 0), stop=(k == 7))
                return last
            cx.op("pe", mmkv, reads=[hTr, wres], writes=[pkr])
            kv, kvr = kvring.next()
            cx.op("act", lambda e: e.copy(out=kv[:], in_=pk[:, :]), reads=[pkr], writes=[kvr])
            cx.op("act", lambda e: e.copy(out=Vall[:, ci, :], in_=kv[:, 256:512]), reads=[kvr], writes=[vres[ci]])
            sq, sqr = sqring.next()
            rs, rsr = rsring.next()
            cx.op("dve", lambda e: e.tensor_tensor(out=sq[:, 0:256], in0=kv[:, 0:256], in1=kv[:, 0:256], op=ALU.mult), reads=[kvr], writes=[sqr])
            cx.op("dve", lambda e: e.tensor_reduce(out=rs[:, 0:4], in_=sq[:, 0:256].rearrange("p (h q) -> p h q", h=4), axis=mybir.AxisListType.X, op=ALU.add),
                  reads=[sqr], writes=[rsr])
            cx.op("dve", lambda e: e.tensor_scalar(out=rs[:, 4:8], in0=rs[:, 0:4], scalar1=1.0 / 64, scalar2=EPS, op0=ALU.mult, op1=ALU.add), reads=[rsr], writes=[rsr])
            cx.op("act", lambda e: e.activation(out=rs[:, 8:12], in_=rs[:, 4:8], func=AF.Sqrt), reads=[rsr], writes=[rsr])
            cx.op("dve", lambda e: e.reciprocal(out=rs[:, 12:16], in_=rs[:, 8:12]), reads=[rsr], writes=[rsr])
            cx.op("dve", lambda e: e.tensor_tensor(out=sq[:, 0:256].rearrange("p (h q) -> p h q", h=4), in0=kv[:, 0:256].rearrange("p (h q) -> p h q", h=4),
                                                   in1=rs[:, 12:16].unsqueeze(2).to_broadcast([128, 4, 64]), op=ALU.mult), reads=[kvr, rsr, sqr], writes=[sqr])
            kd, kdr = kdring.next()
            for dup in range(2):
                cx.op("dve", lambda e: e.tensor_tensor(out=kd[:, :, dup, :], in0=sq[:, 0:256].rearrange("p (h q) -> p h q", h=4),
                                                       in1=kgb[:, :].unsqueeze(1).to_broadcast([128, 4, 64]), op=ALU.mult), reads=[sqr, cres], writes=[kdr])

            def trk(e):
                last = None
                for g in range(4):
                    last = e.transpose(out=tb[:, g * 128:(g + 1) * 128], in_=kd[:, g, :, :].rearrange("p a q -> p (a q)"), identity=K.ident_b)
                return last
            cx.op("pe", trk, reads=[kdr, K.res], writes=[tbr])
            cx.op("act", lambda e: e.copy(out=KT[:, ci, :, :], in_=tb[:, 0:512].rearrange("p (g t) -> p g t", g=4)), reads=[tbr], writes=[kres[ci]])
            if ci == 0:
                continue
            qr, qrr = qrring.next()
            for n in range(2):
                pq, pqr = P[n]

                def mmq(e):
                    last = None
                    for k in range(8):
                        last = e.matmul(pq[:, :], lhsT=hT[:, k, :], rhs=qkvw[:, k, n * 512:(n + 1) * 512], start=(k == 0), stop=(k == 7))
                    return last
                cx.op("pe", mmq, reads=[hTr, wres], writes=[pqr])
                cx.op("act", lambda e: e.copy(out=qr[:, n * 512:(n + 1) * 512], in_=pq[:, :]), reads=[pqr], writes=[qrr])
            cx.op("dve", lambda e: e.tensor_tensor(out=sq[:], in0=qr[:], in1=qr[:], op=ALU.mult), reads=[qrr, sqr], writes=[sqr])
            cx.op("dve", lambda e: e.tensor_reduce(out=rs[:, 16:32], in_=sq[:, :].rearrange("p (h q) -> p h q", h=16), axis=mybir.AxisListType.X, op=ALU.add),
                  reads=[sqr], writes=[rsr])
            cx.op("dve", lambda e: e.tensor_scalar(out=rs[:, 16:32], in0=rs[:, 16:32], scalar1=1.0 / 64, scalar2=EPS, op0=ALU.mult, op1=ALU.add), reads=[rsr], writes=[rsr])
            cx.op("act", lambda e: e.activation(out=rs[:, 16:32], in_=rs[:, 16:32], func=AF.Sqrt), reads=[rsr], writes=[rsr])
            cx.op("dve", lambda e: e.reciprocal(out=rs[:, 16:32], in_=rs[:, 16:32]), reads=[rsr], writes=[rsr])
            cx.op("dve", lambda e: e.tensor_tensor(out=sq[:, :].rearrange("p (h q) -> p h q", h=16), in0=qr[:, :].rearrange("p (h q) -> p h q", h=16),
                                                   in1=rs[:, 16:32].unsqueeze(2).to_broadcast([128, 16, 64]), op=ALU.mult), reads=[qrr, rsr, sqr], writes=[sqr])
            qn, qnr = qnring.next()
            cx.op("dve", lambda e: e.tensor_tensor(out=qn[:, :].rearrange("p (h q) -> p h q", h=16), in0=sq[:, :].rearrange("p (h q) -> p h q", h=16),
                                                   in1=qgb[:, :].unsqueeze(1).to_broadcast([128, 16, 64]), op=ALU.mult), reads=[sqr, cres], writes=[qnr])

            def trq(e):
                last = None
                for k in range(8):
                    last = e.transpose(out=tb[:, k * 128:(k + 1) * 128], in_=qn[:, k * 128:(k + 1) * 128], identity=K.ident_b)
                return last
            cx.op("pe", trq, reads=[qnr, K.res], writes=[tbr])
            qT, qTr = qTring.next()
            cx.op("act", lambda e: e.copy(out=qT[:], in_=tb.rearrange("p (k t) -> p k t", k=8)), reads=[tbr], writes=[qTr])
            ao, aor = aoring.next()
            for hq in range(16):
                pair, half, g = hq // 2, hq % 2, hq // 4
                psc, pscr = P[4 + hq % 2]
                ps_ = slice(half * 64, (half + 1) * 64)
                cx.op("pe", lambda e: e.matmul(psc[:, 0:256], lhsT=qT[ps_, pair, :], rhs=KT[ps_, ci - 1:ci + 1, g, :], start=True, stop=True),
                      reads=[qTr, kres[ci - 1], kres[ci]], writes=[pscr])
                S_, Sr = Sring.next()
                cx.op("dve", lambda e: e.tensor_tensor(out=S_[:], in0=psc[:, 0:256], in1=bias[:, hq, :], op=ALU.add), reads=[pscr, bres], writes=[Sr])
                if ci == 1:
                    cx.op("dve", lambda e: e.tensor_scalar(out=S_[:, 0:128], in0=S_[:, 0:128], scalar1=hneg[:, 0:1], scalar2=None, op0=ALU.add),
                          reads=[Sr, cres], writes=[Sr])
                st, sr = stat.next()
                cx.op("dve", lambda e: e.reduce_max(out=st[:, 0:1], in_=S_[:], axis=mybir.AxisListType.X), reads=[Sr], writes=[sr])
                cx.op("dve", lambda e: e.tensor_tensor(out=st[:, 1:2], in0=st[:, 0:1], in1=snk[:, hq:hq + 1], op=ALU.max), reads=[sr, cres], writes=[sr])
                cx.op("dve", lambda e: e.tensor_scalar(out=st[:, 2:3], in0=st[:, 1:2], scalar1=-1.0, scalar2=None, op0=ALU.mult), reads=[sr], writes=[sr])
                p_, pr = pring.next()
                cx.op("act", lambda e: e.activation(out=p_[:], in_=S_[:], func=AF.Exp, bias=st[:, 2:3], accum_out=st[:, 3:4]), reads=[Sr, sr], writes=[pr, sr])
                cx.op("act", lambda e: e.activation(out=st[:, 4:5], in_=snk[:, hq:hq + 1], func=AF.Exp, bias=st[:, 2:3]), reads=[sr, cres], writes=[sr])
                cx.op("dve", lambda e: e.tensor_tensor(out=st[:, 5:6], in0=st[:, 3:4], in1=st[:, 4:5], op=ALU.add), reads=[sr], writes=[sr])
                cx.op("dve", lambda e: e.reciprocal(out=st[:, 6:7], in_=st[:, 5:6]), reads=[sr], writes=[sr])

                def trp(e):
                    e.transpose(out=tb[:, 0:128], in_=p_[:, 0:128], identity=K.ident_b)
                    return e.transpose(out=tb[:, 128:256], in_=p_[:, 128:256], identity=K.ident_b)
                cx.op("pe", trp, reads=[pr, K.res], writes=[tbr])
                pT, pTr = pTring.next()
                cx.op("act", lambda e: e.copy(out=pT[:], in_=tb[:, 0:256].rearrange("p (k t) -> p k t", k=2)), reads=[tbr], writes=[pTr])
                po, por = P[6 + hq % 2]

                def mmpv(e):
                    e.matmul(po[:, 0:64], lhsT=pT[:, 0, :], rhs=Vall[:, ci - 1, g * 64:(g + 1) * 64], start=True, stop=False)
                    return e.matmul(po[:, 0:64], lhsT=pT[:, 1, :], rhs=Vall[:, ci, g * 64:(g + 1) * 64], start=False, stop=True)
                cx.op("pe", mmpv, reads=[pTr, vres[ci - 1], vres[ci]], writes=[por])
                cx.op("dve", lambda e: e.tensor_scalar(out=ao[:, hq * 64:(hq + 1) * 64], in0=po[:, 0:64], scalar1=st[:, 6:7], scalar2=None, op0=ALU.mult),
                      reads=[por, sr], writes=[aor])

            def tra(e):
                last = None
                for k in range(8):
                    last = e.transpose(out=tb[:, k * 128:(k + 1) * 128], in_=ao[:, k * 128:(k + 1) * 128], identity=K.ident_b)
                return last
            cx.op("pe", tra, reads=[aor, K.res], writes=[tbr])
            aoT, aoTr = aoTring.next()
            cx.op("act", lambda e: e.copy(out=aoT[:], in_=tb.rearrange("p (k t) -> p k t", k=8)), reads=[tbr], writes=[aoTr])
            for half in range(2):
                pf, pfr = P[half]

                def mmo(e):
                    last = None
                    for k in range(8):
                        last = e.matmul(pf[:, :], lhsT=aoT[:, k, :], rhs=outw[:, k, half * 512:(half + 1) * 512], start=(k == 0), stop=(k == 7))
                    return last
                cx.op("pe", mmo, reads=[aoTr, wres], writes=[pfr])
                o, orr = oring.next()
                hs = slice(half * 512, (half + 1) * 512)
                cx.op("dve", lambda e: e.tensor_tensor(out=o[:], in0=pf[:, :], in1=gb[:, hs], op=ALU.mult), reads=[pfr, modres_g], writes=[orr])
                cx.op("dve", lambda e: e.tensor_tensor(out=X[:, ci, hs], in0=X[:, ci, hs], in1=o[:], op=ALU.add), reads=[orr, Xres[ci]], writes=[Xres[ci]])
        cx.barrier()


def build_program():
    nc = bass.Bass("TRN2", target_bir_lowering=False)
    cx = Ctx(nc)

    def din(name, shape, dt=F32):
        return nc.dram_tensor(name, shape, dt, kind="ExternalInput").ap()
    x_in = din("x", [17 * 128, D])
    cT = din("cT", [128, 8])
    ada_w = din("ada_w", [2, D, 6 * D])
    ada_b = din("ada_b", [2, 6 * D])
    n1g = din("n1g", [2, D])
    n2g = din("n2g", [2, D])
    tm_in = din("tmask", [128, 64])
    WM = dict(xpre=din("xpre", [NPRE * 128, D]), inA=din("inA", [4, D, SLABW]), inB=din("inB", [4, D, SLABW]), outw=din("m_outw", [2048, D]),
              cw=din("cw", [128, 24, 4]), cb=din("cb", [128, 24]), dtb=din("dtb", [32]), alog=din("alog", [32]), dsk=din("dsk", [32]), normg=din("normg", [2048]))
    WA = dict(qkvw=din("qkvw", [D, 1536]), outw=din("a_outw", [D, D]), qg=din("qg", [64]), kg=din("kg", [64]), sinks=din("sinks", [16]),
              relb=din("relb", [32, 16]), ohb=din("ohb", [32, 128]))
    rw = din("rw", [2, 128, 8, NE])
    rb = din("rb", [2, NE])
    w1 = din("w1", [2, NE, D, 2 * D])
    w2 = din("w2", [2, NE, D, D])
    b1t = din("b1t", [2, 128, NE, 16])
    b2 = din("b2", [2, NE, D])
    kc = din("kc", [128, 512])
    y = nc.dram_tensor("y", [16 * 128, D], F32, kind="ExternalOutput").ap()
    scr = dict(xg=nc.dram_tensor("xg_scr", [NE * CAP, D], BF16, kind="Internal").ap(),
               yg=nc.dram_tensor("yg_scr", [NE * CAP, D], F32, kind="Internal").ap(), xg_res=Res("xg"), yg_res=Res("yg"))
    rscr = nc.dram_tensor("rscr", [16, 384], F32, kind="Internal").ap()

    X = nc.alloc_sbuf_tensor("X", [128, 17, D], F32)
    Xres = [Res("X%d" % i) for i in range(17)]
    PSB = [(nc.alloc_psum_tensor("ps%d" % i, [128, 512], F32), Res("ps%d" % i, excl=True)) for i in range(8)]
    ps_t = (PSB[7][0][:, :].bitcast(BF16), PSB[7][1])
    K = Consts(cx, kc)
    tmask = nc.alloc_sbuf_tensor("tmask_sb", [128, 64], F32)
    tmres = Res("tm")
    cx.dma("sp", lambda e: e.dma_start(out=tmask[:], in_=tm_in), writes=[tmres])
    xl = Res("xload")
    for c in range(17):
        cx.dma("sp", lambda e: e.dma_start(out=X[:, c, :], in_=x_in[c * 128:(c + 1) * 128, :]), writes=[Xres[c]], sres=xl)
    weff = nc.alloc_sbuf_tensor("weff", [128, D], F32)
    sh = nc.alloc_sbuf_tensor("sh", [128, D], F32)
    gb = nc.alloc_sbuf_tensor("gb", [128, D], F32)
    mres = Res("mod")
    mod = dict(weff=(weff, mres), sh=(sh, mres), g=(gb, mres))
    cx.begin_stage()

    def adaln(layer, first, gain_ap):
        with nc.sbuf_tensor("nb_%d_%d" % (layer, first), [128, D], F32) as nb:
            nres = Res("nb")
            cx.dma("sp", lambda e: e.dma_start(out=nb[:], in_=gain_ap.partition_broadcast(128)), writes=[nres])
            emit_adaln(cx, K, cT, ada_w[layer], ada_b[layer],
                       [(first, sh, mres, "copy", None, None), (first + 1, weff, mres, "scale", nb, nres), (first + 2, gb, mres, "copy", None, None)],
                       PSB[0], PSB[1])
        cx.end_stage()

    def moe(layer, chunks):
        W = dict(rw=rw[layer], rb=rb[layer], w1=w1[layer], w2=w2[layer], b1t=b1t[layer], b2=b2[layer])
        emit_moe(cx, K, X, Xres, chunks, mod, W, (PSB[0:7], ps_t), scr)
        cx.end_stage()

    adaln(0, 0, n1g[0])
    emit_mamba(cx, K, X, Xres, mod, WM, PSB, tmask, tmres)
    cx.end_stage()
    adaln(0, 3, n2g[0])
    moe(0, list(range(17)))
    adaln(1, 0, n1g[1])
    emit_attn(cx, K, X, Xres, mod, WA, PSB, tmask, tmres, rscr)
    cx.end_stage()
    adaln(1, 3, n2g[1])
    moe(1, list(range(1, 17)))
    ores = Res("out")
    for c in range(1, 17):
        cx.dma("sp", lambda e: e.dma_start(out=y[(c - 1) * 128:c * 128, :], in_=X[:, c, :]), reads=[Xres[c]], writes=[ores])
    cx._wait("sp", ores.w)
    return nc


def kernel(x, c, ada_w, ada_b, norm1_g, norm2_g, m_in_w, m_conv_w, m_conv_b, m_dt_bias, m_A_log, m_D, m_norm_g, m_out_w,
           a_qkv_w, a_q_norm_g, a_k_norm_g, a_sinks, a_out_w, rel_bias, r_w, r_b, e_w1, e_b1, e_w2, e_b2):
    f = lambda a: np.ascontiguousarray(np.asarray(a, dtype=np.float32))
    x = f(x); c = f(c)
    e_b1 = np.asarray(e_b1, dtype=np.float32)
    b1p = np.concatenate([e_b1[..., 0::2], e_b1[..., 1::2]], axis=-1)
    b1t = np.ascontiguousarray(b1p.reshape(2, NE, 16, 128).transpose(0, 3, 1, 2))
    rw = np.ascontiguousarray(np.asarray(r_w, dtype=np.float32).reshape(2, 8, 128, NE).transpose(0, 2, 1, 3))
    hm = host_mamba_weights(f(m_in_w)[0], f(m_conv_w)[0], f(m_conv_b)[0])
    common = dict(ada_w=f(ada_w), ada_b=f(ada_b), n1g=f(norm1_g), n2g=f(norm2_g), inA=hm["inA"], inB=hm["inB"], cw=hm["cw"], cb=hm["cb"],
                  m_outw=f(m_out_w)[0], dtb=f(m_dt_bias)[0], alog=f(m_A_log)[0], dsk=f(m_D)[0], normg=f(m_norm_g)[0],
                  qkvw=f(a_qkv_w)[0], a_outw=f(a_out_w)[0], qg=f(a_q_norm_g)[0], kg=f(a_k_norm_g)[0], sinks=f(a_sinks)[0], relb=f(rel_bias),
                  ohb=host_bucket_onehot(), rw=rw, rb=f(r_b), w1=f(e_w1), w2=f(e_w2), b1t=b1t, b2=f(e_b2), kc=host_consts())
    in_maps = []
    for core in range(8):
        b, k = core // 4, core % 4
        m = dict(common)
        m.update(host_core_x(x[b], k))
        m["cT"] = np.ascontiguousarray(c[b].reshape(8, 128).T)
        in_maps.append(m)
    nc = build_program()
    out = np.zeros((2, 8192, D), np.float32)
    for core in range(8):
        b, k = core // 4, core % 4
        res = run_bass_kernel_spmd(nc, [in_maps[core]], core_ids=[0])
        out[b, k * 2048:(k + 1) * 2048] = res.results[0]["y"]
    return out
```
